# Optimizing a Trainium2 kernel written in Bass

```python
import math
import jax, jax.numpy as jnp
from jax import lax
import numpy as np

D_MODEL = 2048
BATCH = 4
SEQ = 2048
DEPTH = 2

GRID_W = 64
CTX_LEN = 256
BLOCK = 128
WINDOW = 128
ROPE_THETA = 10000.0
EPS = 1e-6
NEG_INF = -1e30

A_HEADS = 4
A_DK = 64
A_DV = 2 * A_DK
B_HEADS = 8
B_KV_HEADS = 2
B_GROUP = B_HEADS // B_KV_HEADS
B_DH = 128
C_HEADS = 4
C_Q_RANK = 512
C_KV_RANK = 256
C_NOPE = 128
C_ROPE = 64
C_DQK = C_NOPE + C_ROPE
C_DV = 128

SPLIT_SIZES = (A_HEADS * 2 * A_DK, A_HEADS * 2 * A_DK, A_HEADS * A_DV,
               B_HEADS * B_DH, B_KV_HEADS * B_DH, B_KV_HEADS * B_DH,
               C_Q_RANK, C_KV_RANK, C_ROPE)
D_IN = sum(SPLIT_SIZES)
D_MIX = A_HEADS * A_DV + B_HEADS * B_DH + C_HEADS * C_DV

N_EXPERTS = 32
N_GROUPS = 4
EXPERTS_PER_GROUP = N_EXPERTS // N_GROUPS
TOP_K = 2
GROUP_SCORE_K = 2
D_EXPERT = 512

kernel_name = 'hybrid_diffusion_block'


def rmsnorm(x, g):
    xf = x.astype(jnp.float32)
    y = xf * lax.rsqrt(jnp.mean(xf * xf, axis=-1, keepdims=True) + EPS)
    return (y * g.astype(jnp.float32)).astype(x.dtype)


def modulate(h, shift, scale):
    return h * (1 + scale) + shift


def ada_modulation(cond, w, bias):
    m = jax.nn.silu(cond) @ w + bias
    return jnp.split(m, 6, axis=-1)


def axial_rope(rows, dim):
    t = jnp.arange(rows * GRID_W)
    r = (t // GRID_W).astype(jnp.float32)
    col = (t % GRID_W).astype(jnp.float32)
    nf = dim // 4
    inv = ROPE_THETA ** (-jnp.arange(nf, dtype=jnp.float32) / nf)
    ang = jnp.concatenate([r[:, None] * inv, col[:, None] * inv], axis=-1)
    return jnp.cos(ang), jnp.sin(ang)


def apply_rope(x, cos, sin):
    shape = (cos.shape[0],) + (1,) * (x.ndim - 3) + (cos.shape[-1],)
    cos, sin = cos.reshape(shape), sin.reshape(shape)
    xf = x.astype(jnp.float32)
    x1, x2 = jnp.split(xf, 2, axis=-1)
    return jnp.concatenate([x1 * cos - x2 * sin, x1 * sin + x2 * cos], axis=-1).astype(x.dtype)


def split_cols(z):
    parts, off = [], 0
    for size in SPLIT_SIZES:
        parts.append(z[..., off:off + size])
        off += size
    return parts


def mixer_inputs(h, w_in, a_qn, a_kn, b_qn, b_kn, c_qa_norm, c_kva_norm, c_wuq, c_wukv,
                 c_qn, c_kn, rope):
    bsz, n = h.shape[:2]
    aq, ak, av, bq, bk, bv, cq, ckv, ckr = split_cols(h @ w_in)
    aq = rmsnorm(aq.reshape(bsz, n, A_HEADS, 2, A_DK), a_qn)
    ak = rmsnorm(ak.reshape(bsz, n, A_HEADS, 2, A_DK), a_kn)
    av = av.reshape(bsz, n, A_HEADS, A_DV)
    bq = rmsnorm(bq.reshape(bsz, n, B_HEADS, B_DH), b_qn)
    bk = rmsnorm(bk.reshape(bsz, n, B_KV_HEADS, B_DH), b_kn)
    bv = bv.reshape(bsz, n, B_KV_HEADS, B_DH)
    q = (rmsnorm(cq, c_qa_norm) @ c_wuq).reshape(bsz, n, C_HEADS, C_DQK)
    kv = (rmsnorm(ckv, c_kva_norm) @ c_wukv).reshape(bsz, n, C_HEADS, C_NOPE + C_DV)
    q_nope = rmsnorm(q[..., :C_NOPE], c_qn[:C_NOPE])
    q_rope = rmsnorm(q[..., C_NOPE:], c_qn[C_NOPE:])
    k_nope = rmsnorm(kv[..., :C_NOPE], c_kn[:C_NOPE])
    cv = kv[..., C_NOPE:]
    k_rope = rmsnorm(ckr, c_kn[C_NOPE:])
    if rope is not None:
        (cos_a, sin_a), (cos_b, sin_b), (cos_c, sin_c) = rope
        aq, ak = apply_rope(aq, cos_a, sin_a), apply_rope(ak, cos_a, sin_a)
        bq, bk = apply_rope(bq, cos_b, sin_b), apply_rope(bk, cos_b, sin_b)
        q_rope = apply_rope(q_rope, cos_c, sin_c)
        k_rope = apply_rope(k_rope, cos_c, sin_c)
    cq_full = jnp.concatenate([q_nope, q_rope], axis=-1)
    ck_full = jnp.concatenate(
        [k_nope, jnp.broadcast_to(k_rope[:, :, None, :], (bsz, n, C_HEADS, C_ROPE))], axis=-1)
    return aq, ak, av, bq, bk, bv, cq_full, ck_full, cv


def sweep_blocks(fn, q):
    bsz, n = q.shape[:2]
    nb = n // BLOCK
    qb = jnp.moveaxis(q.reshape((bsz, nb, BLOCK) + q.shape[2:]), 1, 0)
    ob = lax.map(fn, qb)
    return jnp.moveaxis(ob, 0, 1).reshape((bsz, n) + ob.shape[3:])


def diff_lambda(lv, layer_idx):
    lam_init = 0.8 - 0.6 * math.exp(-0.3 * layer_idx)
    lv = lv.astype(jnp.float32)
    lam = jnp.exp(jnp.sum(lv[0] * lv[1])) - jnp.exp(jnp.sum(lv[2] * lv[3])) + lam_init
    return lam, lam_init


def diff_attention(q, k, v, lam):
    s = jnp.einsum('bqhmd,bkhmd->bhmqk', q, k).astype(jnp.float32) * (A_DK ** -0.5)
    p = jax.nn.softmax(s, axis=-1)
    p = p[:, :, 0] - lam * p[:, :, 1]
    return jnp.einsum('bhqk,bkhd->bqhd', p.astype(v.dtype), v)


def mha(q, k, v, scale):
    s = jnp.einsum('bqhd,bkhd->bhqk', q, k).astype(jnp.float32) * scale
    p = jax.nn.softmax(s, axis=-1)
    return jnp.einsum('bhqk,bkhd->bqhd', p.astype(v.dtype), v)


def swa_latent(q, k, v, k_ctx, v_ctx, sink):
    bsz, n = q.shape[:2]
    nb = n // BLOCK
    qb = q.reshape(bsz, nb, BLOCK, B_KV_HEADS, B_GROUP, B_DH)

    def band(t):
        tp = jnp.pad(t, ((0, 0), (BLOCK, BLOCK), (0, 0), (0, 0)))
        tp = tp.reshape(bsz, nb + 2, BLOCK, B_KV_HEADS, B_DH)
        return jnp.concatenate([tp[:, :nb], tp[:, 1:nb + 1], tp[:, 2:]], axis=2)

    kb, vb = band(k), band(v)
    qpos = jnp.arange(nb)[:, None] * BLOCK + jnp.arange(BLOCK)[None, :]
    kpos = (jnp.arange(nb)[:, None] - 1) * BLOCK + jnp.arange(3 * BLOCK)[None, :]
    valid = ((jnp.abs(qpos[:, :, None] - kpos[:, None, :]) <= WINDOW)
             & (kpos[:, None, :] >= 0) & (kpos[:, None, :] < n))
    scale = B_DH ** -0.5
    s_loc = jnp.einsum('bnqhgd,bnkhd->bnhgqk', qb, kb).astype(jnp.float32) * scale
    s_loc = jnp.where(valid[None, :, None, None], s_loc, NEG_INF)
    s_ctx = jnp.einsum('bnqhgd,bchd->bnhgqc', qb, k_ctx).astype(jnp.float32) * scale
    s_sink = jnp.broadcast_to(
        sink.astype(jnp.float32).reshape(1, 1, B_KV_HEADS, B_GROUP, 1, 1), s_loc.shape[:-1] + (1,))
    p = jax.nn.softmax(jnp.concatenate([s_loc, s_ctx, s_sink], axis=-1), axis=-1)
    n_loc = 3 * BLOCK
    p_loc = p[..., :n_loc].astype(v.dtype)
    p_ctx = p[..., n_loc:n_loc + k_ctx.shape[1]].astype(v.dtype)
    out = (jnp.einsum('bnhgqk,bnkhd->bnqhgd', p_loc, vb)
           + jnp.einsum('bnhgqc,bchd->bnqhgd', p_ctx, v_ctx))
    return out.reshape(bsz, n, B_HEADS, B_DH)


def swa_context(q, k, v, sink):
    bsz, n = q.shape[:2]
    qg = q.reshape(bsz, n, B_KV_HEADS, B_GROUP, B_DH)
    s = jnp.einsum('bqhgd,bkhd->bhgqk', qg, k).astype(jnp.float32) * (B_DH ** -0.5)
    s_sink = jnp.broadcast_to(
        sink.astype(jnp.float32).reshape(1, B_KV_HEADS, B_GROUP, 1, 1), s.shape[:-1] + (1,))
    p = jax.nn.softmax(jnp.concatenate([s, s_sink], axis=-1), axis=-1)[..., :-1]
    out = jnp.einsum('bhgqk,bkhd->bqhgd', p.astype(v.dtype), v)
    return out.reshape(bsz, n, B_HEADS, B_DH)


def moe(h, router_w, router_bias, w_gate, w_up, w_down):
    t = h.shape[0]
    scores = jax.nn.sigmoid(h.astype(jnp.float32) @ router_w.astype(jnp.float32))
    sel = scores + router_bias.astype(jnp.float32)
    gscore = lax.top_k(sel.reshape(t, N_GROUPS, EXPERTS_PER_GROUP), GROUP_SCORE_K)[0].sum(-1)
    gidx = jnp.argmax(gscore, axis=-1)
    in_group = (jnp.arange(N_EXPERTS) // EXPERTS_PER_GROUP)[None, :] == gidx[:, None]
    _, eidx = lax.top_k(jnp.where(in_group, sel, NEG_INF), TOP_K)
    w = jnp.take_along_axis(scores, eidx, axis=-1)
    w = w / jnp.sum(w, axis=-1, keepdims=True)
    gates = jnp.sum(jax.nn.one_hot(eidx, N_EXPERTS, dtype=jnp.float32) * w[..., None], axis=1)
    gates = gates.astype(h.dtype)
    y = jnp.zeros_like(h)
    for e in range(N_EXPERTS):
        a = jax.nn.silu(h @ w_gate[e]) * (h @ w_up[e])
        y = y + gates[:, e:e + 1] * (a @ w_down[e])
    return y


def setup_inputs(seed: int = 0) -> dict:
    key = jax.random.key(seed)
    keys = iter(jax.random.split(key, 40))

    def nrm(shape, scale):
        return jax.random.normal(next(keys), shape, jnp.float32) * scale

    def gain(shape):
        return 1.0 + nrm(shape, 0.05)

    D = D_MODEL
    return {
        'x': nrm((BATCH, SEQ, D), 1.0),
        'c': nrm((BATCH, D), 1.0),
        'ctx': nrm((BATCH, CTX_LEN, D), 1.0),
        'c_ctx': nrm((D,), 1.0),
        'ada_w': nrm((DEPTH, D, 6 * D), 0.5 * D ** -0.5),
        'ada_b': nrm((DEPTH, 6 * D), 0.01),
        'norm1_g': gain((DEPTH, D)),
        'norm2_g': gain((DEPTH, D)),
        'w_in': nrm((DEPTH, D, D_IN), D ** -0.5),
        'w_out': nrm((DEPTH, D_MIX, D), D_MIX ** -0.5),
        'a_qn': gain((DEPTH, A_DK)),
        'a_kn': gain((DEPTH, A_DK)),
        'a_lambda': nrm((DEPTH, 4, A_DK), 0.1),
        'a_subln': gain((DEPTH, A_DV)),
        'b_qn': gain((DEPTH, B_DH)),
        'b_kn': gain((DEPTH, B_DH)),
        'b_sink': nrm((DEPTH, B_HEADS), 1.0),
        'c_qa_norm': gain((DEPTH, C_Q_RANK)),
        'c_kva_norm': gain((DEPTH, C_KV_RANK)),
        'c_wuq': nrm((DEPTH, C_Q_RANK, C_HEADS * C_DQK), C_Q_RANK ** -0.5),
        'c_wukv': nrm((DEPTH, C_KV_RANK, C_HEADS * (C_NOPE + C_DV)), C_KV_RANK ** -0.5),
        'c_qn': gain((DEPTH, C_DQK)),
        'c_kn': gain((DEPTH, C_DQK)),
        'router_w': nrm((D, N_EXPERTS), D ** -0.5),
        'router_bias': nrm((N_EXPERTS,), 0.01),
        'moe_w_gate': nrm((DEPTH, N_EXPERTS, D, D_EXPERT), D ** -0.5),
        'moe_w_up': nrm((DEPTH, N_EXPERTS, D, D_EXPERT), D ** -0.5),
        'moe_w_down': nrm((DEPTH, N_EXPERTS, D_EXPERT, D), D_EXPERT ** -0.5),
    }


def reference(x, c, ctx, c_ctx, ada_w, ada_b, norm1_g, norm2_g, w_in, w_out,
              a_qn, a_kn, a_lambda, a_subln, b_qn, b_kn, b_sink,
              c_qa_norm, c_kva_norm, c_wuq, c_wukv, c_qn, c_kn,
              router_w, router_bias, moe_w_gate, moe_w_up, moe_w_down):
    bsz, n_lat, d = x.shape
    n_ctx = ctx.shape[1]
    rows = n_lat // GRID_W
    rope = (axial_rope(rows, A_DK), axial_rope(rows, B_DH), axial_rope(rows, C_ROPE))
    xc = ctx
    for l in range(DEPTH):
        last = l == DEPTH - 1
        sh1, sc1, g1, sh2, sc2, g2 = [m[:, None, :] for m in ada_modulation(c, ada_w[l], ada_b[l])]
        sh1c, sc1c, g1c, sh2c, sc2c, g2c = ada_modulation(c_ctx, ada_w[l], ada_b[l])
        lp = (w_in[l], a_qn[l], a_kn[l], b_qn[l], b_kn[l], c_qa_norm[l], c_kva_norm[l],
              c_wuq[l], c_wukv[l], c_qn[l], c_kn[l])

        h = modulate(rmsnorm(x, norm1_g[l]), sh1, sc1)
        hc = modulate(rmsnorm(xc, norm1_g[l]), sh1c, sc1c)
        aq, ak, av, bq, bk, bv, cq, ck, cv = mixer_inputs(h, *lp, rope=rope)
        aqc, akc, avc, bqc, bkc, bvc, cqc, ckc, cvc = mixer_inputs(hc, *lp, rope=None)
        lam, lam_init = diff_lambda(a_lambda[l], l)

        ak_all, av_all = jnp.concatenate([ak, akc], axis=1), jnp.concatenate([av, avc], axis=1)
        ck_all, cv_all = jnp.concatenate([ck, ckc], axis=1), jnp.concatenate([cv, cvc], axis=1)
        o_a = sweep_blocks(lambda qb: diff_attention(qb, ak_all, av_all, lam), aq)
        o_a = rmsnorm(o_a, a_subln[l]) * (1.0 - lam_init)
        o_b = swa_latent(bq, bk, bv, bkc, bvc, b_sink[l])
        o_c = sweep_blocks(lambda qb: mha(qb, ck_all, cv_all, C_DQK ** -0.5), cq)
        mix = jnp.concatenate([o_a.reshape(bsz, n_lat, -1), o_b.reshape(bsz, n_lat, -1),
                               o_c.reshape(bsz, n_lat, -1)], axis=-1)
        x = x + g1 * (mix @ w_out[l])

        if not last:
            oc_a = rmsnorm(diff_attention(aqc, akc, avc, lam), a_subln[l]) * (1.0 - lam_init)
            oc_b = swa_context(bqc, bkc, bvc, b_sink[l])
            oc_c = mha(cqc, ckc, cvc, C_DQK ** -0.5)
            mixc = jnp.concatenate([oc_a.reshape(bsz, n_ctx, -1), oc_b.reshape(bsz, n_ctx, -1),
                                    oc_c.reshape(bsz, n_ctx, -1)], axis=-1)
            xc = xc + g1c * (mixc @ w_out[l])

        h2 = modulate(rmsnorm(x, norm2_g[l]), sh2, sc2).reshape(bsz * n_lat, d)
        if not last:
            h2c = modulate(rmsnorm(xc, norm2_g[l]), sh2c, sc2c).reshape(bsz * n_ctx, d)
            y = moe(jnp.concatenate([h2, h2c], axis=0), router_w, router_bias,
                    moe_w_gate[l], moe_w_up[l], moe_w_down[l])
            y_lat = y[:bsz * n_lat].reshape(bsz, n_lat, d)
            xc = xc + g2c * y[bsz * n_lat:].reshape(bsz, n_ctx, d)
        else:
            y_lat = moe(h2, router_w, router_bias, moe_w_gate[l], moe_w_up[l],
                        moe_w_down[l]).reshape(bsz, n_lat, d)
        x = x + g2 * y_lat
    return x
```

```python
import math
import os
from contextlib import ExitStack
import numpy as np
import concourse.bass as bass
import concourse.mybir as mybir
from concourse.bass_utils import run_bass_kernel_spmd

F32 = mybir.dt.float32
BF16 = mybir.dt.bfloat16
AF = mybir.ActivationFunctionType
ALU = mybir.AluOpType
AX = mybir.AxisListType

D = 2048
SEQ = 2048
NCTX = 256
DEPTH = 2
D_IN = 3904
NE = 32
DE = 512
EPS = 1e-6
CH = 12000


class U:
    __slots__ = ("w", "r", "ps")

    def __init__(self, ps=False):
        self.w = None
        self.r = {}
        self.ps = ps


class KB:
    def __init__(self, nc, st):
        self.nc = nc
        self.st = st
        self.h = {"pe": nc.tensor, "act": nc.scalar, "dve": nc.vector, "pool": nc.gpsimd, "sp": nc.sync}
        self.cnt = {e: 0 for e in self.h}
        self.sems = {e: [] for e in self.h}
        self.known = {e: {} for e in self.h}
        self.dq = {}
        self.ndma = 0
        self.n = 0
        self.limit = int(os.environ.get("KLIMIT", "1000000000"))
        self.log = []

    def _sem(self, chain, idx):
        lst = self.sems.setdefault(chain, [])
        ch = 600 if chain.startswith("dma") else CH
        k = (idx - 1) // ch
        while len(lst) <= k:
            lst.append(self.st.enter_context(self.nc.semaphore(f"s_{chain}_{len(lst)}")))
        return lst[k], (idx - 1) % ch + 1

    def _wait(self, eng, tok):
        chain, idx = tok
        if chain == eng == "pe":
            return
        if self.known[eng].get(chain, 0) >= idx:
            return
        self.known[eng][chain] = idx
        sem, val = self._sem(chain, idx)
        self.h[eng].wait_ge(sem, val * 16 if chain.startswith("dma") else val)

    def _deps(self, eng, r, w):
        toks = []
        for u in r:
            if u.w is not None:
                toks.append(u.w)
        for u in w:
            if u.w is not None:
                toks.append(u.w)
            toks.extend(u.r.items())
        for t in toks:
            self._wait(eng, t)

    def _mark(self, tok, r, w):
        for u in r:
            if u.r.get(tok[0], 0) < tok[1]:
                u.r[tok[0]] = tok[1]
        for u in w:
            u.w = tok
            u.r = {}

    def op(self, eng, fn, r=(), w=()):
        self.n += 1
        if self.n > self.limit:
            return None
        if any(u.ps for u in r):
            w = list(w) + [u for u in r if u.ps]
            r = [u for u in r if not u.ps]
        self._deps(eng, r, w)
        ins = fn(self.h[eng])
        self.cnt[eng] += 1
        idx = self.cnt[eng]
        sem, val = self._sem(eng, idx)
        ins.then_inc(sem, 1)
        self._mark((eng, idx), r, w)
        return ins

    def dma(self, q, out, in_, r=(), w=()):
        self.n += 1
        if self.n > self.limit:
            return None
        self._deps(q, r, w)
        ins = self.h[q].dma_start(out=out, in_=in_)
        self.ndma += 1
        slot = self.dq.setdefault(q, {"n": 0})
        chain = f"dma_{q}_{slot['n'] % 6}"
        slot["n"] += 1
        c = self.cnt.get(chain, 0) + 1
        self.cnt[chain] = c
        sem, val = self._sem(chain, c)
        ins.then_inc(sem, 16)
        self._mark((chain, c), r, w)
        return ins

    def barrier(self):
        self.log.append(self.n)
        toks = [(c, n) for c, n in self.cnt.items() if n > 0]
        for e in self.h:
            for t in toks:
                if t[0] != e:
                    self._wait(e, t)


def _dma_ch_fix():
    pass


def _layer_program(last, debug=False, maxphase=9):
    nc = bass.Bass("TRN2", target_bir_lowering=False)
    NQT = 8 if last else 10
    NQ = NQT * 128
    QT = list(range(8)) + ([] if last else [16, 17])
    NKT = 18
    NK = NKT * 128

    def din(name, shape):
        return nc.dram_tensor(name, list(shape), F32, kind="ExternalInput").ap()

    xs = din("xs", [NK, D])
    cT = din("cT", [128, 16, 2])
    ada_w = din("ada_w", [D, 6 * D])
    ada_b = din("ada_b", [1, 6 * D])
    n1g = din("norm1_g", [1, D])
    n2g = din("norm2_g", [1, D])
    w_in = din("w_in", [D, D_IN])
    w_out = din("w_out", [D, D])
    a_qn = din("a_qn", [1, 64]); a_kn = din("a_kn", [1, 64])
    a_lambda = din("a_lambda", [1, 256]); a_subln = din("a_subln", [1, 128])
    b_qn = din("b_qn", [1, 128]); b_kn = din("b_kn", [1, 128]); b_sink = din("b_sink", [1, 8])
    c_qa = din("c_qa_norm", [1, 512]); c_kva = din("c_kva_norm", [1, 256])
    c_wuq = din("c_wuq", [512, 768]); c_wukv = din("c_wukv", [256, 1024])
    c_qn = din("c_qn", [1, 192]); c_kn = din("c_kn", [1, 192])
    router_w = din("router_w", [D, NE]); router_b = din("router_bias", [1, NE])
    wg = din("moe_w_gate", [NE, D, DE]); wu = din("moe_w_up", [NE, D, DE]); wd = din("moe_w_down", [NE, DE, D])
    ident_d = din("ident", [128, 128])
    masks_d = din("masks", [4, 128, 128])
    ropeA = din("ropeA", [2, SEQ, 32]); ropeB = din("ropeB", [2, SEQ, 64]); ropeC = din("ropeC", [2, SEQ, 32])
    laminit = din("laminit", [1, 2])

    xo = nc.dram_tensor("xo", [NQ, D], F32, kind="ExternalOutput").ap()

    def dtmp(name, shape, dt=BF16, out=False):
        if debug:
            return nc.dram_tensor(name, list(shape), dt, kind="ExternalOutput").ap()
        return nc.dram_tensor(name, list(shape), dt).ap()
    modv = dtmp("modv", [2, 6 * D], F32, out=True)
    x1d = dtmp("x1d", [NQ, D], F32, out=True)
    mixd = dtmp("mixd", [NQ, D], BF16, out=True) if debug else None
    gated = dtmp("gated", [NQ, NE], F32, out=True) if debug else None
    AKT = dtmp("AKT", [64, 8, NK]); AQT = dtmp("AQT", [64, 8, NQ]); AVd = dtmp("AVd", [NK, 512])
    BKT = dtmp("BKT", [128, 2, NK]); BQT = dtmp("BQT", [128, 8, NQ]); BVd = dtmp("BVd", [NK, 256])
    CKT = dtmp("CKT", [128, 4, NK]); CKRT = dtmp("CKRT", [64, NK]); CQT = dtmp("CQT", [128, 4, NQ])
    CQRT = dtmp("CQRT", [64, 4, NQ]); CVd = dtmp("CVd", [NK, 512])

    with ExitStack() as st:
        kb = KB(nc, st)
        nm = [0]

        def sb(name, shape, dt=F32, stack=None):
            nm[0] += 1
            return (stack or st).enter_context(nc.sbuf_tensor(f"{name}_{nm[0]}", list(shape), dt))

        def ps(name, shape, dt=F32, stack=None):
            nm[0] += 1
            return (stack or st).enter_context(nc.psum_tensor(f"{name}_{nm[0]}", list(shape), dt))

        ident = sb("ident", [128, 128], BF16); u_ident = U()
        kb.dma("pool", ident[:], ident_d, w=[u_ident])
        epsb = sb("epsb", [128, 1], F32); u_epsb = U()
        kb.op("dve", lambda e: e.memset(epsb[:], EPS), w=[u_epsb])
        EPSB = epsb[:, 0:1]
        gates = sb("gates", [128, NQT, NE], F32)
        u_gates = [U() for _ in range(NQT)]
        h2T = sb("h2T", [128, 16, NQ], BF16); u_h2T = [U() for _ in range(NQT)]

        with ExitStack() as p0:
            cTs = sb("cTs", [128, 16, 2], F32, p0); u_c = U()
            cTb = sb("cTb", [128, 16, 2], BF16, p0); u_cb = U()
            kb.dma("sp", cTs[:], cT, w=[u_c])
            kb.op("act", lambda e: e.activation(out=cTb[:], in_=cTs[:], func=AF.Silu), r=[u_c], w=[u_cb])
            NB = 3
            wbuf = [sb(f"adw{i}", [128, 16, 512], BF16, p0) for i in range(NB)]
            u_wb = [U() for _ in range(NB)]
            bias = [sb(f"adb{i}", [2, 512], F32, p0) for i in range(2)]; u_bias = [U(), U()]
            mrow = sb("mrow", [2, 6 * D], F32, p0); u_mrow = U()
            pm = [ps(f"pm{i}", [128, 512], F32, p0) for i in range(2)]
            u_pm = [U(True), U(True)]
            for j in range(24):
                s = j % NB
                kb.dma("pool", wbuf[s][:], ada_w[:, j * 512:(j + 1) * 512].rearrange("(k p) n -> p k n", p=128),
                       w=[u_wb[s]])
                kb.dma("sp", bias[j % 2][:], ada_b[:, j * 512:(j + 1) * 512].partition_broadcast(2), w=[u_bias[j % 2]])
                pj = pm[j % 2]
                for k in range(16):
                    kb.op("pe", lambda e, k=k, s=s, pj=pj: e.matmul(pj[0:2, :], lhsT=cTb[:, k, :], rhs=wbuf[s][:, k, :],
                                                                     start=(k == 0), stop=(k == 15)),
                          r=[u_cb, u_wb[s]], w=[u_pm[j % 2]])
                kb.op("dve", lambda e, j=j, pj=pj: e.tensor_tensor(out=mrow[:, j * 512:(j + 1) * 512], in0=pj[0:2, :],
                                                                     in1=bias[j % 2][:], op=ALU.add),
                      r=[u_pm[j % 2], u_bias[j % 2]], w=[u_mrow])
            kb.dma("sp", modv, mrow[:], r=[u_mrow])
        kb.barrier()

        def load_mod(tile, which, idx, u):
            kb.dma("sp", tile[:], modv[which:which + 1, idx * D:(idx + 1) * D].partition_broadcast(128), w=[u])

        def bc_row(tile, src, u, q="sp"):
            kb.dma(q, tile[:], src.partition_broadcast(128), w=[u])

        def headnorm(src, H, d, gain, out, r, w, scr, u_scr):
            sq, ssum = scr
            sqv = sq[:, 0:H * d].rearrange("p (h d) -> p h d", h=H)
            kb.op("act", lambda e: e.activation(out=sqv, in_=src, func=AF.Square), r=r, w=[u_scr[0]])
            kb.op("dve", lambda e: e.tensor_reduce(out=ssum[:, 0:H], in_=sqv, axis=AX.X, op=ALU.add),
                  r=[u_scr[0]], w=[u_scr[1]])
            kb.op("act", lambda e: e.activation(out=ssum[:, 8:8 + H], in_=ssum[:, 0:H], func=AF.Sqrt, scale=1.0 / d, bias=EPSB),
                  r=[u_scr[1], u_epsb], w=[u_scr[1]])
            kb.op("dve", lambda e: e.reciprocal(out=ssum[:, 0:H], in_=ssum[:, 8:8 + H]), r=[u_scr[1]], w=[u_scr[1]])
            kb.op("dve", lambda e: e.tensor_tensor(out=sqv, in0=src,
                                                   in1=ssum[:, 0:H].unsqueeze(2).broadcast_to([128, H, d]),
                                                   op=ALU.mult), r=r + [u_scr[1]], w=[u_scr[0]])
            kb.op("dve", lambda e: e.tensor_tensor(out=out, in0=sqv,
                                                   in1=gain.unsqueeze(1).broadcast_to([128, H, d]),
                                                   op=ALU.mult), r=[u_scr[0]], w=w)

        if maxphase < 1:
            return nc
        with ExitStack() as p1:
            wbuf = sb("w_in_sb", [128, 16, 2048], BF16, p1); u_win = U()
            for k in range(16):
                rs_ = slice(k * 128, (k + 1) * 128)
                kb.dma("pool", wbuf[:, k, 0:1024], w_in[rs_, 512:1536], w=[u_win])
                kb.dma("pool", wbuf[:, k, 1024:1536], w_in[rs_, 2560:3072], w=[u_win])
                kb.dma("pool", wbuf[:, k, 1536:1856], w_in[rs_, 3584:3904], w=[u_win])
            wuq_sb = sb("wuq_sb", [128, 4, 768], BF16, p1); u_wuq = U()
            kb.dma("pool", wuq_sb[:], c_wuq.rearrange("(k p) n -> p k n", p=128), w=[u_wuq])
            wukv_sb = sb("wukv_sb", [128, 2, 1024], BF16, p1); u_wukv = U()
            kb.dma("pool", wukv_sb[:], c_wukv.rearrange("(k p) n -> p k n", p=128), w=[u_wukv])
            Gt = sb("G", [128, D], F32, p1); SHt = sb("SH", [128, D], F32, p1)
            u_G = U(); u_SH = U()
            junk = sb("junk", [128, D], F32, p1); u_junk = U()

            def set_mod(which):
                bc_row(junk, n1g, u_junk)
                load_mod(Gt, which, 1, u_G)
                load_mod(SHt, which, 0, u_SH)
                kb.op("dve", lambda e: e.scalar_tensor_tensor(out=Gt[:], in0=Gt[:], scalar=1.0, in1=junk[:],
                                                              op0=ALU.add, op1=ALU.mult), r=[u_junk], w=[u_G])

            gsm = sb("gsm", [128, 1664], F32, p1); u_gsm = U()
            off = {}
            o = 0
            for name, src, n in [("a_qn", a_qn, 64), ("a_kn", a_kn, 64), ("b_qn", b_qn, 128), ("b_kn", b_kn, 128),
                                 ("c_qa", c_qa, 512), ("c_kva", c_kva, 256), ("c_qn", c_qn, 192), ("c_kn", c_kn, 192)]:
                off[name] = (o, n)
                kb.dma("sp", gsm[:, o:o + n], src.partition_broadcast(128), w=[u_gsm])
                o += n

            def gain(name, lo=0, hi=None):
                o, n = off[name]
                return gsm[:, o + lo:o + (n if hi is None else hi)]

            xt = sb("xt", [128, D], F32, p1); u_xt = U()
            st1 = sb("st1", [128, 4], F32, p1); u_st1 = U()
            hb = sb("hb", [128, D], BF16, p1); u_hb = U()
            hT = [sb(f"hT{i}", [128, 16, 128], BF16, p1) for i in range(2)]; u_hT = [U(), U()]
            sq = sb("sq", [128, 512], F32, p1); ssum = sb("ssum", [128, 16], F32, p1)
            u_scr = [U(), U()]
            nrm = sb("nrm", [128, 512], F32, p1); u_nrm = U()
            rt = sb("rt", [128, 4, 256], F32, p1); u_rt = U()
            zb = sb("zb", [128, 1024], BF16, p1); u_zb = U()
            vb = sb("vb", [128, 1024], BF16, p1); u_vb = [U(), U(), U()]
            cn = sb("cn", [128, 512], BF16, p1); u_cn = U()
            cnT = sb("cnT", [128, 4, 128], BF16, p1); u_cnT = U()
            tT = [sb(f"tT{i}", [128, 8, 128], BF16, p1) for i in range(2)]; u_tT = [U(), U()]
            ropes = sb("ropes", [128, 2, 160], F32, p1); u_ropes = U()
            pT = [ps(f"pT{i}", [128, 8, 128], BF16, p1) for i in range(2)]; u_pT = [U(True), U(True)]
            pz = [ps(f"pz{i}", [128, 512], F32, p1) for i in range(4)]; u_pz = [U(True) for _ in range(4)]
            pzc = [0]
            ptc = [0]
            ttc = [0]

            def next_pz():
                i = pzc[0] % 4
                pzc[0] += 1
                return pz[i], u_pz[i]

            def rope(src, H, d, tab_off, out, r, w):
                h2 = d // 2
                cos = ropes[:, 0, tab_off:tab_off + h2].unsqueeze(1).broadcast_to([128, H, h2])
                sin = ropes[:, 1, tab_off:tab_off + h2].unsqueeze(1).broadcast_to([128, H, h2])
                x1 = src[:, :, 0:h2]; x2 = src[:, :, h2:d]
                t = [rt[:, i, 0:H * h2].rearrange("p (h d) -> p h d", h=H) for i in range(4)]
                kb.op("dve", lambda e: e.tensor_tensor(out=t[0], in0=x1, in1=cos, op=ALU.mult), r=r + [u_ropes], w=[u_rt])
                kb.op("dve", lambda e: e.tensor_tensor(out=t[1], in0=x2, in1=sin, op=ALU.mult), r=r + [u_ropes], w=[u_rt])
                kb.op("dve", lambda e: e.tensor_tensor(out=t[2], in0=x1, in1=sin, op=ALU.mult), r=r + [u_ropes], w=[u_rt])
                kb.op("dve", lambda e: e.tensor_tensor(out=t[3], in0=x2, in1=cos, op=ALU.mult), r=r + [u_ropes], w=[u_rt])
                kb.op("dve", lambda e: e.tensor_tensor(out=out[:, :, 0:h2], in0=t[0], in1=t[1], op=ALU.subtract), r=[u_rt], w=w)
                kb.op("dve", lambda e: e.tensor_tensor(out=out[:, :, h2:d], in0=t[2], in1=t[3], op=ALU.add), r=[u_rt], w=w)

            def to_dram_T(src, nblk, rows, dst_fn, r):
                i = ptc[0] % 2
                ptc[0] += 1
                for j in range(nblk):
                    kb.op("pe", lambda e, j=j: e.transpose(out=pT[i][0:rows, j, :], in_=src(j), identity=ident[:]),
                          r=r + [u_ident], w=[u_pT[i]])
                k = ttc[0] % 2
                ttc[0] += 1
                kb.op("act", lambda e: e.copy(out=tT[k][0:rows, 0:nblk, :], in_=pT[i][0:rows, 0:nblk, :]),
                      r=[u_pT[i]], w=[u_tT[k]])
                for j in range(nblk):
                    kb.dma("sp", dst_fn(j), tT[k][0:rows, j, :], r=[u_tT[k]])

            def load_ropes(t):
                for (a, b_, tab) in ((0, 32, ropeA), (32, 96, ropeB), (96, 128, ropeC)):
                    kb.dma("sp", ropes[:, :, a:b_], tab[:, t * 128:(t + 1) * 128, :].rearrange("c p f -> p c f"), w=[u_ropes])

            def proj(hTap, u_h, c0, n):
                p, up = next_pz()
                for k in range(16):
                    kb.op("pe", lambda e, k=k, p=p: e.matmul(p[:, 0:n], lhsT=hTap(k), rhs=wbuf[:, k, c0:c0 + n],
                                                             start=(k == 0), stop=(k == 15)),
                          r=[u_h, u_win], w=[up])
                return p, up

            def norm_rope_T(is_ctx, p, up, n, H, d, gname, tab_off, dst_fn, rows, c0=0):
                src = p[:, c0:c0 + n].rearrange("p (h d) -> p h d", h=H)
                g = gain(gname)
                zv = zb[:, 0:n].rearrange("p (h d) -> p h d", h=H)
                if is_ctx:
                    headnorm(src, H, d, g, zv, [up], [u_zb], (sq, ssum), u_scr)
                else:
                    nv = nrm[:, 0:n].rearrange("p (h d) -> p h d", h=H)
                    headnorm(src, H, d, g, nv, [up], [u_nrm], (sq, ssum), u_scr)
                    rope(nv, H, d, tab_off, zv, [u_nrm], [u_zb])
                to_dram_T(lambda j: zb[:, j * rows:(j + 1) * rows], n // rows, rows, dst_fn, [u_zb])

            for t in range(NKT):
                is_ctx = t >= 16
                if t == 0:
                    set_mod(0)
                if t == 16:
                    set_mod(1)
                is_q = t in QT
                qi = QT.index(t) if is_q else None
                kb.dma("sp", xt[:], xs[t * 128:(t + 1) * 128, :], w=[u_xt])
                if not is_ctx:
                    load_ropes(t)
                kb.op("act", lambda e: e.activation(out=junk[:], in_=xt[:], func=AF.Square, accum_out=st1[:, 0:1]),
                      r=[u_xt], w=[u_junk, u_st1])
                kb.op("act", lambda e: e.activation(out=st1[:, 1:2], in_=st1[:, 0:1], func=AF.Sqrt, scale=1.0 / D, bias=EPSB), r=[u_st1, u_epsb], w=[u_st1])
                kb.op("dve", lambda e: e.reciprocal(out=st1[:, 2:3], in_=st1[:, 1:2]), r=[u_st1], w=[u_st1])
                kb.op("dve", lambda e: e.scalar_tensor_tensor(out=junk[:], in0=xt[:], scalar=st1[:, 2:3],
                                                              in1=Gt[:], op0=ALU.mult, op1=ALU.mult),
                      r=[u_xt, u_st1, u_G], w=[u_junk])
                kb.op("dve", lambda e: e.tensor_tensor(out=hb[:], in0=junk[:], in1=SHt[:], op=ALU.add),
                      r=[u_junk, u_SH], w=[u_hb])
                hTt = hT[t % 2]; u_hTt = u_hT[t % 2]
                for half in range(2):
                    i = ptc[0] % 2
                    ptc[0] += 1
                    for j in range(8):
                        c = half * 8 + j
                        kb.op("pe", lambda e, j=j, c=c, i=i: e.transpose(out=pT[i][:, j, :], in_=hb[:, c * 128:(c + 1) * 128],
                                                                          identity=ident[:]),
                              r=[u_hb, u_ident], w=[u_pT[i]])
                    kb.op("act", lambda e, half=half, i=i, hTt=hTt: e.copy(out=hTt[:, half * 8:(half + 1) * 8, :], in_=pT[i][:]),
                          r=[u_pT[i]], w=[u_hTt])
                    if is_q:
                        kb.op("dve", lambda e, half=half, i=i, qi=qi: e.tensor_copy(
                            out=h2T[:, half * 8:(half + 1) * 8, qi * 128:(qi + 1) * 128], in_=pT[i][:]),
                              r=[u_pT[i]], w=[u_h2T[qi]])
                hap = lambda k, hTt=hTt: hTt[:, k, :]
                ts = slice(t * 128, (t + 1) * 128)
                p, up = proj(hap, u_hTt, 0, 512)
                norm_rope_T(is_ctx, p, up, 512, 8, 64, "a_kn", 0, lambda j: AKT[:, j, ts], 64)
                p, up = proj(hap, u_hTt, 512, 512)
                kb.op("act", lambda e, p=p: e.copy(out=vb[:, 0:512], in_=p[:, 0:512]), r=[up], w=[u_vb[0]])
                kb.dma("sp", AVd[ts, :], vb[:, 0:512], r=[u_vb[0]])
                p, up = proj(hap, u_hTt, 1024, 512)
                kb.op("act", lambda e, p=p: e.copy(out=vb[:, 512:768], in_=p[:, 256:512]), r=[up], w=[u_vb[1]])
                kb.dma("sp", BVd[ts, :], vb[:, 512:768], r=[u_vb[1]])
                norm_rope_T(is_ctx, p, up, 256, 2, 128, "b_kn", 32, lambda j: BKT[:, j, ts], 128)
                p, up = proj(hap, u_hTt, 1536, 320)
                src = p[:, 0:256].rearrange("p (h d) -> p h d", h=1)
                headnorm(src, 1, 256, gain("c_kva"), cn[:, 0:256].rearrange("p (h d) -> p h d", h=1), [up], [u_cn],
                         (sq, ssum), u_scr)
                srcr = p[:, 256:320].rearrange("p (h d) -> p h d", h=1)
                zvr = zb[:, 512:576].rearrange("p (h d) -> p h d", h=1)
                gkr = gsm[:, off["c_kn"][0] + 128:off["c_kn"][0] + 192]
                if is_ctx:
                    headnorm(srcr, 1, 64, gkr, zvr, [up], [u_zb], (sq, ssum), u_scr)
                else:
                    nv = nrm[:, 0:64].rearrange("p (h d) -> p h d", h=1)
                    headnorm(srcr, 1, 64, gkr, nv, [up], [u_nrm], (sq, ssum), u_scr)
                    rope(nv, 1, 64, 96, zvr, [u_nrm], [u_zb])
                to_dram_T(lambda j: zb[:, 512:576], 1, 64, lambda j: CKRT[:, ts], [u_zb])
                i = ptc[0] % 2; ptc[0] += 1
                for j in range(2):
                    kb.op("pe", lambda e, j=j, i=i: e.transpose(out=pT[i][:, j, :], in_=cn[:, j * 128:(j + 1) * 128],
                                                                 identity=ident[:]), r=[u_cn, u_ident], w=[u_pT[i]])
                kb.op("act", lambda e, i=i: e.copy(out=cnT[:, 0:2, :], in_=pT[i][:, 0:2, :]), r=[u_pT[i]], w=[u_cnT])
                gkn = gsm[:, off["c_kn"][0]:off["c_kn"][0] + 128]
                for hp in range(2):
                    p2, up2 = next_pz()
                    for k in range(2):
                        kb.op("pe", lambda e, k=k, p2=p2, hp=hp: e.matmul(p2[:, 0:512], lhsT=cnT[:, k, :],
                                                                           rhs=wukv_sb[:, k, hp * 512:(hp + 1) * 512],
                                                                           start=(k == 0), stop=(k == 1)),
                              r=[u_cnT, u_wukv], w=[up2])
                    kvv = p2[:, 0:512].rearrange("p (h d) -> p h d", h=2)
                    zv = zb[:, 0:256].rearrange("p (h d) -> p h d", h=2)
                    headnorm(kvv[:, :, 0:128], 2, 128, gkn, zv, [up2], [u_zb], (sq, ssum), u_scr)
                    to_dram_T(lambda j: zb[:, j * 128:(j + 1) * 128], 2, 128,
                              lambda j, hp=hp: CKT[:, hp * 2 + j, ts], [u_zb])
                    vv = vb[:, 768:1024].rearrange("p (h d) -> p h d", h=2)
                    kb.op("act", lambda e, kvv=kvv, vv=vv: e.copy(out=vv, in_=kvv[:, :, 128:256]), r=[up2], w=[u_vb[2]])
                    kb.dma("sp", CVd[ts, hp * 256:(hp + 1) * 256], vb[:, 768:1024], r=[u_vb[2]])

            for k in range(16):
                rs_ = slice(k * 128, (k + 1) * 128)
                kb.dma("pool", wbuf[:, k, 0:512], w_in[rs_, 0:512], w=[u_win])
                kb.dma("pool", wbuf[:, k, 512:1536], w_in[rs_, 1536:2560], w=[u_win])
                kb.dma("pool", wbuf[:, k, 1536:2048], w_in[rs_, 3072:3584], w=[u_win])
            for qi in range(NQT):
                t = QT[qi]
                is_ctx = t >= 16
                if not is_ctx:
                    load_ropes(t)
                qs = slice(qi * 128, (qi + 1) * 128)
                hap = lambda k, qs=qs: h2T[:, k, qs]
                uh = u_h2T[qi]
                p, up = proj(hap, uh, 0, 512)
                norm_rope_T(is_ctx, p, up, 512, 8, 64, "a_qn", 0, lambda j: AQT[:, j, qs], 64)
                for hh in range(2):
                    p, up = proj(hap, uh, 512 + hh * 512, 512)
                    norm_rope_T(is_ctx, p, up, 512, 4, 128, "b_qn", 32, lambda j, hh=hh: BQT[:, hh * 4 + j, qs], 128)
                p, up = proj(hap, uh, 1536, 512)
                src = p[:, 0:512].rearrange("p (h d) -> p h d", h=1)
                headnorm(src, 1, 512, gain("c_qa"), cn[:, 0:512].rearrange("p (h d) -> p h d", h=1), [up], [u_cn],
                         (sq, ssum), u_scr)
                i = ptc[0] % 2; ptc[0] += 1
                for j in range(4):
                    kb.op("pe", lambda e, j=j, i=i: e.transpose(out=pT[i][:, j, :], in_=cn[:, j * 128:(j + 1) * 128],
                                                                 identity=ident[:]), r=[u_cn, u_ident], w=[u_pT[i]])
                kb.op("act", lambda e, i=i: e.copy(out=cnT[:, 0:4, :], in_=pT[i][:, 0:4, :]), r=[u_pT[i]], w=[u_cnT])
                gq0 = gsm[:, off["c_qn"][0]:off["c_qn"][0] + 128]
                gq1 = gsm[:, off["c_qn"][0] + 128:off["c_qn"][0] + 192]
                for hp in range(2):
                    p2, up2 = next_pz()
                    for k in range(4):
                        kb.op("pe", lambda e, k=k, p2=p2, hp=hp: e.matmul(p2[:, 0:384], lhsT=cnT[:, k, :],
                                                                           rhs=wuq_sb[:, k, hp * 384:(hp + 1) * 384],
                                                                           start=(k == 0), stop=(k == 3)),
                              r=[u_cnT, u_wuq], w=[up2])
                    qv = p2[:, 0:384].rearrange("p (h d) -> p h d", h=2)
                    zv = zb[:, 0:256].rearrange("p (h d) -> p h d", h=2)
                    headnorm(qv[:, :, 0:128], 2, 128, gq0, zv, [up2], [u_zb], (sq, ssum), u_scr)
                    to_dram_T(lambda j: zb[:, j * 128:(j + 1) * 128], 2, 128,
                              lambda j, hp=hp: CQT[:, hp * 2 + j, qs], [u_zb])
                    zv2 = zb[:, 256:384].rearrange("p (h d) -> p h d", h=2)
                    if is_ctx:
                        headnorm(qv[:, :, 128:192], 2, 64, gq1, zv2, [up2], [u_zb], (sq, ssum), u_scr)
                    else:
                        nv = nrm[:, 0:128].rearrange("p (h d) -> p h d", h=2)
                        headnorm(qv[:, :, 128:192], 2, 64, gq1, nv, [up2], [u_nrm], (sq, ssum), u_scr)
                        rope(nv, 2, 64, 96, zv2, [u_nrm], [u_zb])
                    to_dram_T(lambda j: zb[:, 256 + j * 64:256 + (j + 1) * 64], 2, 64,
                              lambda j, hp=hp: CQRT[:, hp * 2 + j, qs], [u_zb])
        kb.barrier()

        with ExitStack() as att:
            mix = sb("mix", [128, NQT, D], BF16, att)
            u_mix = [U() for _ in range(NQT)]
            if maxphase < 2:
                print("KB ops at barriers", kb.log)
                return nc
            with ExitStack() as p2s:
                KT = sb("KT", [128, 4, NK], BF16, p2s); u_KT = U()
                KRT = sb("KRT", [64, NK], BF16, p2s); u_KRT = U()
                QTs = sb("QTs", [128, 8, NQ], BF16, p2s); u_QT = U()
                QRT = sb("QRT", [64, 4, NQ], BF16, p2s); u_QRT = U()
                V = sb("V", [128, NKT, 4, 132], BF16, p2s); u_V = U()
                PT = [sb(f"PT{i}", [128, 512], BF16, p2s) for i in range(3)]; u_PT = [U() for _ in range(3)]
                msk = sb("msk", [128, 4, 128], BF16, p2s); u_msk = U()
                kb.dma("pool", msk[:], masks_d.rearrange("m p f -> p m f"), w=[u_msk])
                cst = sb("cst", [128, 512], F32, p2s); u_cst = U()
                kb.dma("sp", cst[:, 0:256], a_lambda.partition_broadcast(128), w=[u_cst])
                kb.dma("sp", cst[:, 256:384], a_subln.partition_broadcast(128), w=[u_cst])
                kb.dma("sp", cst[:, 384:392], b_sink.partition_broadcast(128), w=[u_cst])
                kb.dma("sp", cst[:, 392:394], laminit.partition_broadcast(128), w=[u_cst])
                lv = cst[:, 0:256].rearrange("p (a d) -> p a d", a=4)
                C = dict(r=[u_cst], w=[u_cst])
                kb.op("dve", lambda e: e.tensor_tensor(out=cst[:, 416:480], in0=cst[:, 0:64], in1=cst[:, 64:128], op=ALU.mult), **C)
                kb.op("dve", lambda e: e.tensor_reduce(out=cst[:, 400:401], in_=cst[:, 416:480], axis=AX.X, op=ALU.add), **C)
                kb.op("dve", lambda e: e.tensor_tensor(out=cst[:, 416:480], in0=cst[:, 128:192], in1=cst[:, 192:256], op=ALU.mult), **C)
                kb.op("dve", lambda e: e.tensor_reduce(out=cst[:, 401:402], in_=cst[:, 416:480], axis=AX.X, op=ALU.add), **C)
                kb.op("act", lambda e: e.activation(out=cst[:, 402:404], in_=cst[:, 400:402], func=AF.Exp), **C)
                kb.op("dve", lambda e: e.tensor_tensor(out=cst[:, 404:405], in0=cst[:, 403:404], in1=cst[:, 402:403],
                                                       op=ALU.subtract), **C)
                kb.op("dve", lambda e: e.tensor_tensor(out=cst[:, 405:406], in0=cst[:, 404:405], in1=cst[:, 392:393],
                                                       op=ALU.subtract), **C)
                NEGLAM = cst[:, 405:406]
                kb.op("dve", lambda e: e.tensor_scalar(out=cst[:, 256:384], in0=cst[:, 256:384], scalar1=cst[:, 393:394],
                                                       scalar2=None, op0=ALU.mult), **C)
                kb.op("act", lambda e: e.activation(out=cst[:, 408:416], in_=cst[:, 384:392], func=AF.Exp), **C)
                ESINK = 408
                fin = sb("fin", [128, 4, 128], F32, p2s); u_fin = U()
                fsc = sb("fsc", [128, 16], F32, p2s); u_fsc = U()
                psS = [ps(f"psS{i}", [128, 512], F32, p2s) for i in range(3)]; u_psS = [U(True) for _ in range(3)]
                psO = [ps(f"psO{i}", [128, 3, 132], F32, p2s) for i in range(3)]
                u_psO = [U(True) for _ in range(3)]
                started = set()

                def acc(i):
                    return psO[i // 3][:, i % 3, 0:129], u_psO[i // 3]

                def acc_start(i, first):
                    if not first:
                        return False
                    b = i // 3
                    if b in started:
                        return False
                    started.add(b)
                    return True

                kb.op("pool", lambda e: e.memset(V[:, :, :, 128:129], 1.0), w=[u_V])
                sc_cnt = [0]

                def scoreT(kparts, qparts, n, scale, maskidx=None, r=(), nh=1):
                    i = sc_cnt[0] % 3
                    sc_cnt[0] += 1
                    o_ap = psS[i][:, 0:n] if nh == 1 else psS[i][:, 0:n].rearrange("p (h q) -> p h q", h=nh)
                    for j, (l, rr) in enumerate(zip(kparts, qparts)):
                        kb.op("pe", lambda e, l=l, rr=rr, j=j: e.matmul(o_ap, lhsT=l, rhs=rr, start=(j == 0),
                                                                         stop=(j == len(kparts) - 1)),
                              r=list(r), w=[u_psS[i]])
                    kb.op("act", lambda e: e.activation(out=PT[i][:, 0:n], in_=psS[i][:, 0:n], func=AF.Exp, scale=scale),
                          r=[u_psS[i]], w=[u_PT[i]])
                    if maskidx is not None:
                        nhh = n // 128
                        pv = PT[i][:, 0:n].rearrange("p (h q) -> p h q", h=nhh)
                        kb.op("dve", lambda e: e.tensor_tensor(out=pv, in0=pv,
                                                               in1=msk[:, maskidx:maskidx + 1, :].broadcast_to([128, nhh, 128]),
                                                               op=ALU.mult), r=[u_PT[i], u_msk], w=[u_PT[i]])
                    return PT[i], u_PT[i]

                kb.dma("sp", KT[0:64, 0:4, :], AKT[:, 0:4, :], w=[u_KT])
                kb.dma("sp", KT[64:128, 0:4, :], AKT[:, 4:8, :], w=[u_KT])
                kb.dma("sp", QTs[0:64, 0:4, :], AQT[:, 0:4, :], w=[u_QT])
                kb.dma("sp", QTs[64:128, 0:4, :], AQT[:, 4:8, :], w=[u_QT])
                for kt in range(NKT):
                    kb.dma("sp", V[:, kt, :, 0:128], AVd[kt * 128:(kt + 1) * 128, :].rearrange("p (h d) -> p h d", h=4),
                           w=[u_V])

                def a_ops(mh):
                    return (slice(0, 64), mh) if mh < 4 else (slice(64, 128), mh - 4)

                blocks = [(0, 512, list(range(NKT))), (512, 512, list(range(NKT)))]
                if not last:
                    blocks.append((1024, 256, [16, 17]))
                for (q0, qn, keys) in blocks:
                    nqs = qn // 128
                    for h in range(4):
                        started.clear()
                        for ki, kt in enumerate(keys):
                            for m in range(2):
                                psl, idx = a_ops(2 * h + m)
                                P, uP = scoreT([KT[psl, idx, kt * 128:(kt + 1) * 128]], [QTs[psl, idx, q0:q0 + qn]], qn,
                                               1.0 / 8.0, r=[u_KT, u_QT])
                                for qs_ in range(nqs):
                                    a, ua = acc(m * 4 + qs_)
                                    st_ = acc_start(m * 4 + qs_, ki == 0)
                                    kb.op("pe", lambda e, a=a, P=P, qs_=qs_, kt=kt, h=h, ki=ki: e.matmul(
                                        a, lhsT=P[:, qs_ * 128:(qs_ + 1) * 128], rhs=V[:, kt, h, 0:129],
                                        start=st_, stop=(ki == len(keys) - 1), skip_group_check=True), r=[uP, u_V], w=[ua])
                        for qs_ in range(nqs):
                            qt = (q0 // 128) + qs_
                            a0, ua0 = acc(qs_); a1, ua1 = acc(4 + qs_)
                            kb.op("dve", lambda e, a0=a0: e.reciprocal(out=fsc[:, 0:1], in_=a0[:, 128:129]), r=[ua0], w=[u_fsc])
                            kb.op("dve", lambda e, a1=a1: e.reciprocal(out=fsc[:, 1:2], in_=a1[:, 128:129]), r=[ua1], w=[u_fsc])
                            kb.op("dve", lambda e: e.tensor_tensor(out=fsc[:, 2:3], in0=fsc[:, 1:2], in1=NEGLAM, op=ALU.mult),
                                  r=[u_fsc, u_cst], w=[u_fsc])
                            kb.op("dve", lambda e, a0=a0: e.tensor_scalar(out=fin[:, 0, :], in0=a0[:, 0:128], scalar1=fsc[:, 0:1],
                                                                          scalar2=None, op0=ALU.mult), r=[ua0, u_fsc], w=[u_fin])
                            kb.op("dve", lambda e, a1=a1: e.scalar_tensor_tensor(out=fin[:, 1, :], in0=a1[:, 0:128],
                                                                                 scalar=fsc[:, 2:3], in1=fin[:, 0, :],
                                                                                 op0=ALU.mult, op1=ALU.add),
                                  r=[ua1, u_fsc, u_fin], w=[u_fin])
                            kb.op("act", lambda e: e.activation(out=fin[:, 2, :], in_=fin[:, 1, :], func=AF.Square,
                                                                accum_out=fsc[:, 3:4]), r=[u_fin], w=[u_fin, u_fsc])
                            kb.op("act", lambda e: e.activation(out=fsc[:, 4:5], in_=fsc[:, 3:4], func=AF.Sqrt, scale=1.0 / 128, bias=EPSB), r=[u_fsc, u_epsb], w=[u_fsc])
                            kb.op("dve", lambda e: e.reciprocal(out=fsc[:, 5:6], in_=fsc[:, 4:5]), r=[u_fsc], w=[u_fsc])
                            kb.op("dve", lambda e, qt=qt, h=h: e.scalar_tensor_tensor(
                                out=mix[:, qt, h * 128:(h + 1) * 128], in0=fin[:, 1, :], scalar=fsc[:, 5:6],
                                in1=cst[:, 256:384], op0=ALU.mult, op1=ALU.mult), r=[u_fin, u_fsc, u_cst], w=[u_mix[qt]])

                kb.dma("sp", KT[:, 0:2, :], BKT, w=[u_KT])
                kb.dma("sp", QTs[:], BQT, w=[u_QT])
                for kt in range(NKT):
                    kb.dma("sp", V[:, kt, 0:2, 0:128], BVd[kt * 128:(kt + 1) * 128, :].rearrange("p (h d) -> p h d", h=2),
                           w=[u_V])
                sB = 128.0 ** -0.5
                for qt in range(NQT):
                    tok_t = QT[qt]
                    if tok_t >= 16:
                        chunks = [(16, None), (17, None)]
                    else:
                        chunks = [(qt - 1, 0) if qt > 0 else (15, 2), (qt, None), (qt + 1, 1) if qt < 7 else (8, 3),
                                  (16, None), (17, None)]
                    for g in range(2):
                        started.clear()
                        for ci, (kt, mk) in enumerate(chunks):
                            P, uP = scoreT([KT[:, g, kt * 128:(kt + 1) * 128]],
                                           [QTs[:, 4 * g:4 * g + 4, qt * 128:(qt + 1) * 128]],
                                           512, sB, maskidx=mk, r=[u_KT, u_QT], nh=4)
                            for j in range(4):
                                a, ua = acc(j)
                                st_ = acc_start(j, ci == 0)
                                kb.op("pe", lambda e, a=a, P=P, j=j, kt=kt, g=g, ci=ci: e.matmul(
                                    a, lhsT=P[:, j * 128:(j + 1) * 128], rhs=V[:, kt, g, 0:129],
                                    start=st_, stop=(ci == len(chunks) - 1), skip_group_check=True), r=[uP, u_V], w=[ua])
                        for j in range(4):
                            hd = 4 * g + j
                            a, ua = acc(j)
                            kb.op("dve", lambda e, a=a, hd=hd: e.tensor_tensor(out=fsc[:, 0:1], in0=a[:, 128:129],
                                                                               in1=cst[:, ESINK + hd:ESINK + hd + 1], op=ALU.add),
                                  r=[ua, u_cst], w=[u_fsc])
                            kb.op("dve", lambda e: e.reciprocal(out=fsc[:, 1:2], in_=fsc[:, 0:1]), r=[u_fsc], w=[u_fsc])
                            kb.op("dve", lambda e, a=a, hd=hd, qt=qt: e.tensor_scalar(
                                out=mix[:, qt, 512 + hd * 128:512 + (hd + 1) * 128], in0=a[:, 0:128], scalar1=fsc[:, 1:2],
                                scalar2=None, op0=ALU.mult), r=[ua, u_fsc], w=[u_mix[qt]])

                kb.dma("sp", KT[:], CKT, w=[u_KT])
                kb.dma("sp", KRT[:], CKRT, w=[u_KRT])
                kb.dma("sp", QTs[:, 0:4, :], CQT, w=[u_QT])
                kb.dma("sp", QRT[:], CQRT, w=[u_QRT])
                for kt in range(NKT):
                    kb.dma("sp", V[:, kt, :, 0:128], CVd[kt * 128:(kt + 1) * 128, :].rearrange("p (h d) -> p h d", h=4),
                           w=[u_V])
                sC = 192.0 ** -0.5
                for (q0, qn, keys) in blocks:
                    nqs = qn // 128
                    for h in range(4):
                        started.clear()
                        for ki, kt in enumerate(keys):
                            P, uP = scoreT([KT[:, h, kt * 128:(kt + 1) * 128], KRT[:, kt * 128:(kt + 1) * 128]],
                                           [QTs[:, h, q0:q0 + qn], QRT[:, h, q0:q0 + qn]], qn, sC,
                                           r=[u_KT, u_QT, u_KRT, u_QRT])
                            for qs_ in range(nqs):
                                a, ua = acc(qs_)
                                st_ = acc_start(qs_, ki == 0)
                                kb.op("pe", lambda e, a=a, P=P, qs_=qs_, kt=kt, h=h, ki=ki: e.matmul(
                                    a, lhsT=P[:, qs_ * 128:(qs_ + 1) * 128], rhs=V[:, kt, h, 0:129],
                                    start=st_, stop=(ki == len(keys) - 1), skip_group_check=True), r=[uP, u_V], w=[ua])
                        for qs_ in range(nqs):
                            qt = (q0 // 128) + qs_
                            a, ua = acc(qs_)
                            kb.op("dve", lambda e, a=a: e.reciprocal(out=fsc[:, 0:1], in_=a[:, 128:129]), r=[ua], w=[u_fsc])
                            kb.op("dve", lambda e, a=a, qt=qt, h=h: e.tensor_scalar(
                                out=mix[:, qt, 1536 + h * 128:1536 + (h + 1) * 128], in0=a[:, 0:128], scalar1=fsc[:, 0:1],
                                scalar2=None, op0=ALU.mult), r=[ua, u_fsc], w=[u_mix[qt]])
                if debug:
                    for qt in range(NQT):
                        kb.dma("sp", mixd[qt * 128:(qt + 1) * 128, :], mix[:, qt, :], r=[u_mix[qt]])
            kb.barrier()

            if maxphase < 3:
                return nc
            with ExitStack() as p3:
                w_out_sb = sb("w_out_sb", [128, 16, D], BF16, p3); u_wout = U()
                for k in range(16):
                    kb.dma("pool", w_out_sb[:, k, :], w_out[k * 128:(k + 1) * 128, :], w=[u_wout])
                g1b = sb("g1b", [128, D], F32, p3); u_g1b = U()
                mT = sb("mT", [128, 16, 128], BF16, p3); u_mT = U()
                xr = sb("xr", [128, D], F32, p3); u_xr = U()
                x1 = sb("x1", [128, D], F32, p3); u_x1 = U()
                junk = sb("junk3", [128, D], F32, p3); u_junk = U()
                pT = [ps(f"pT3{i}", [128, 8, 128], BF16, p3) for i in range(2)]; u_pT = [U(True), U(True)]
                py = [ps(f"py3{i}", [128, 512], F32, p3) for i in range(4)]; u_py = [U(True) for _ in range(4)]
                ptc3 = 0
                for qt in range(NQT):
                    tok_t = QT[qt]
                    if qt == 0:
                        load_mod(g1b, 0, 2, u_g1b)
                    if tok_t == 16:
                        load_mod(g1b, 1, 2, u_g1b)
                    kb.dma("sp", xr[:], xs[tok_t * 128:(tok_t + 1) * 128, :], w=[u_xr])
                    for half in range(2):
                        i = ptc3 % 2; ptc3 += 1
                        for j in range(8):
                            c = half * 8 + j
                            kb.op("pe", lambda e, j=j, c=c, i=i, qt=qt: e.transpose(out=pT[i][:, j, :],
                                                                                    in_=mix[:, qt, c * 128:(c + 1) * 128],
                                                                                    identity=ident[:]),
                                  r=[u_mix[qt], u_ident], w=[u_pT[i]])
                        kb.op("act", lambda e, half=half, i=i: e.copy(out=mT[:, half * 8:(half + 1) * 8, :], in_=pT[i][:]),
                              r=[u_pT[i]], w=[u_mT])
                    for dc in range(4):
                        for k in range(16):
                            kb.op("pe", lambda e, k=k, dc=dc: e.matmul(py[dc][:], lhsT=mT[:, k, :],
                                                                       rhs=w_out_sb[:, k, dc * 512:(dc + 1) * 512],
                                                                       start=(k == 0), stop=(k == 15)),
                                  r=[u_mT, u_wout], w=[u_py[dc]])
                        dsl = slice(dc * 512, (dc + 1) * 512)
                        kb.op("dve", lambda e, dc=dc, dsl=dsl: e.tensor_tensor(out=junk[:, dsl], in0=py[dc][:],
                                                                                in1=g1b[:, dsl], op=ALU.mult),
                              r=[u_py[dc], u_g1b], w=[u_junk])
                        kb.op("pool", lambda e, dsl=dsl: e.tensor_tensor(out=x1[:, dsl], in0=junk[:, dsl], in1=xr[:, dsl],
                                                                          op=ALU.add), r=[u_junk, u_xr], w=[u_x1])
                    kb.dma("sp", x1d[qt * 128:(qt + 1) * 128, :], x1[:], r=[u_x1])
        kb.barrier()

        if maxphase < 4:
            return nc
        with ExitStack() as p3:
            G2 = sb("G2", [128, D], F32, p3); u_G2 = U()
            SH2 = sb("SH2", [128, D], F32, p3); u_SH2 = U()
            junk = sb("junk3b", [128, D], F32, p3); u_junk = U()

            def set_mod2(which):
                bc_row(junk, n2g, u_junk)
                load_mod(G2, which, 4, u_G2)
                load_mod(SH2, which, 3, u_SH2)
                kb.op("dve", lambda e: e.scalar_tensor_tensor(out=G2[:], in0=G2[:], scalar=1.0, in1=junk[:],
                                                              op0=ALU.add, op1=ALU.mult), r=[u_junk], w=[u_G2])
            rw = sb("rw", [128, 16, NE], BF16, p3); u_rw = U()
            kb.dma("pool", rw[:], router_w.rearrange("(k p) n -> p k n", p=128), w=[u_rw])
            rbias = sb("rbias", [128, NE], F32, p3); u_rb = U()
            bc_row(rbias, router_b, u_rb)
            x1 = [sb(f"x1b{i}", [128, D], F32, p3) for i in range(2)]; u_x1 = [U(), U()]
            hb = sb("hb3", [128, D], BF16, p3); u_hb = U()
            st3 = sb("st3", [128, 4], F32, p3); u_st3 = U()
            rs = sb("rs", [128, 8, NE], F32, p3); u_rs = U()
            rs2 = sb("rs2", [128, 8, 4], F32, p3); u_rs2 = U()
            pT = [ps(f"pT3b{i}", [128, 8, 128], BF16, p3) for i in range(2)]; u_pT = [U(True), U(True)]
            pr = ps("pr3", [128, 512], F32, p3); u_pr = U(True)
            ptc3 = 0
            for qt in range(NQT):
                tok_t = QT[qt]
                if qt == 0:
                    set_mod2(0)
                if tok_t == 16:
                    set_mod2(1)
                b = qt % 2
                kb.dma("sp", x1[b][:], x1d[qt * 128:(qt + 1) * 128, :], w=[u_x1[b]])
                kb.op("act", lambda e, b=b: e.activation(out=junk[:], in_=x1[b][:], func=AF.Square, accum_out=st3[:, 0:1]),
                      r=[u_x1[b]], w=[u_junk, u_st3])
                kb.op("act", lambda e: e.activation(out=st3[:, 1:2], in_=st3[:, 0:1], func=AF.Sqrt, scale=1.0 / D, bias=EPSB), r=[u_st3, u_epsb], w=[u_st3])
                kb.op("dve", lambda e: e.reciprocal(out=st3[:, 2:3], in_=st3[:, 1:2]), r=[u_st3], w=[u_st3])
                kb.op("dve", lambda e, b=b: e.scalar_tensor_tensor(out=junk[:], in0=x1[b][:], scalar=st3[:, 2:3],
                                                                   in1=G2[:], op0=ALU.mult, op1=ALU.mult),
                      r=[u_x1[b], u_st3, u_G2], w=[u_junk])
                kb.op("dve", lambda e: e.tensor_tensor(out=hb[:], in0=junk[:], in1=SH2[:], op=ALU.add),
                      r=[u_junk, u_SH2], w=[u_hb])
                for half in range(2):
                    i = ptc3 % 2; ptc3 += 1
                    for j in range(8):
                        c = half * 8 + j
                        kb.op("pe", lambda e, j=j, c=c, i=i: e.transpose(out=pT[i][:, j, :], in_=hb[:, c * 128:(c + 1) * 128],
                                                                          identity=ident[:]), r=[u_hb, u_ident], w=[u_pT[i]])
                    kb.op("act", lambda e, half=half, i=i, qt=qt: e.copy(
                        out=h2T[:, half * 8:(half + 1) * 8, qt * 128:(qt + 1) * 128], in_=pT[i][:]),
                          r=[u_pT[i]], w=[u_h2T[qt]])
                for k in range(16):
                    kb.op("pe", lambda e, k=k, qt=qt: e.matmul(pr[:, 0:NE], lhsT=h2T[:, k, qt * 128:(qt + 1) * 128], rhs=rw[:, k, :],
                                                               start=(k == 0), stop=(k == 15)), r=[u_h2T[qt], u_rw], w=[u_pr])
                SC = rs[:, 0, :]; SEL = rs[:, 1, :]; T1 = rs[:, 2, :]; T2 = rs[:, 3, :]; MK = rs[:, 4, :]
                g3 = lambda ap: ap.rearrange("p (g e) -> p g e", g=4)
                M1 = rs2[:, 0, :]; M2 = rs2[:, 1, :]; GS = rs2[:, 2, :]; GM = rs2[:, 3, 0:1]; GSEL = rs2[:, 4, :]
                WS = rs2[:, 5, 0:1]; WR = rs2[:, 6, 0:1]
                bc8 = lambda ap: ap.unsqueeze(2).broadcast_to([128, 4, 8])
                kb.op("act", lambda e: e.activation(out=SC, in_=pr[:, 0:NE], func=AF.Sigmoid), r=[u_pr], w=[u_rs])
                R = dict(r=[u_rs, u_rs2], w=[u_rs, u_rs2])
                kb.op("dve", lambda e: e.tensor_tensor(out=SEL, in0=SC, in1=rbias[:], op=ALU.add), r=[u_rs, u_rb], w=[u_rs])
                kb.op("dve", lambda e: e.tensor_reduce(out=M1, in_=g3(SEL), axis=AX.X, op=ALU.max), **R)
                kb.op("dve", lambda e: e.tensor_tensor(out=g3(T1), in0=g3(SEL), in1=bc8(M1), op=ALU.is_equal), **R)
                kb.op("dve", lambda e: e.scalar_tensor_tensor(out=T2, in0=T1, scalar=-1e30, in1=SEL, op0=ALU.mult, op1=ALU.add), **R)
                kb.op("dve", lambda e: e.tensor_reduce(out=M2, in_=g3(T2), axis=AX.X, op=ALU.max), **R)
                kb.op("dve", lambda e: e.tensor_tensor(out=GS, in0=M1, in1=M2, op=ALU.add), **R)
                kb.op("dve", lambda e: e.tensor_reduce(out=GM, in_=GS, axis=AX.X, op=ALU.max), **R)
                kb.op("dve", lambda e: e.tensor_scalar(out=GSEL, in0=GS, scalar1=GM, scalar2=None, op0=ALU.is_equal), **R)
                kb.op("dve", lambda e: e.tensor_tensor(out=g3(MK), in0=g3(T2), in1=bc8(M2), op=ALU.is_equal), **R)
                kb.op("dve", lambda e: e.tensor_tensor(out=MK, in0=MK, in1=T1, op=ALU.add), **R)
                kb.op("dve", lambda e: e.tensor_tensor(out=g3(MK), in0=g3(MK), in1=bc8(GSEL), op=ALU.mult), **R)
                kb.op("dve", lambda e: e.tensor_tensor(out=T1, in0=MK, in1=SC, op=ALU.mult), **R)
                kb.op("dve", lambda e: e.tensor_reduce(out=WS, in_=T1, axis=AX.X, op=ALU.add), **R)
                kb.op("dve", lambda e: e.reciprocal(out=WR, in_=WS), **R)
                kb.op("dve", lambda e, qt=qt: e.tensor_scalar(out=gates[:, qt, :], in0=T1, scalar1=WR, scalar2=None, op0=ALU.mult),
                      r=[u_rs, u_rs2], w=[u_gates[qt]])
                if debug:
                    kb.dma("sp", gated[qt * 128:(qt + 1) * 128, :], gates[:, qt, :], r=[u_gates[qt]])
        kb.barrier()

        if maxphase < 5:
            return nc
        with ExitStack() as p4:
            Y = sb("Y", [128, NQT, D], F32, p4)
            u_Y = [[U() for _ in range(4)] for _ in range(NQT)]
            for qt in range(NQT):
                kb.op("pool", lambda e, qt=qt: e.memset(Y[:, qt, :], 0.0), w=u_Y[qt])
            with ExitStack() as p4a:
                NWB = 2
                wgs = [sb(f"wgs{i}", [128, 16, 256], BF16, p4a) for i in range(NWB)]
                wus = [sb(f"wus{i}", [128, 16, 256], BF16, p4a) for i in range(NWB)]
                wds = [sb(f"wds{i}", [128, 2, D], BF16, p4a) for i in range(NWB)]
                u_wg = [U() for _ in range(NWB)]; u_wu = [U() for _ in range(NWB)]; u_wd = [U() for _ in range(NWB)]
                aT = [sb(f"aT{i}", [128, 2, NQ], BF16, p4a) for i in range(2)]; u_aT = [U(), U()]
                sg = [sb(f"sg{i}", [128, 512], F32, p4a) for i in range(2)]; u_sg = [U(), U()]
                pg = [ps(f"pg{i}", [128, 512], F32, p4a) for i in range(2)]; u_pg = [U(True), U(True)]
                pu = [ps(f"pu{i}", [128, 512], F32, p4a) for i in range(2)]; u_pu = [U(True), U(True)]
                pyy = [ps(f"pyy{i}", [128, 512], F32, p4a) for i in range(4)]; u_pyy = [U(True) for _ in range(4)]
                tbs = [(0, 512), (512, 512)] + ([] if last else [(1024, 256)])
                cnt = 0
                pyc = 0
                for eh in range(NE * 2):
                    e_, half = eh // 2, eh % 2
                    s = eh % NWB
                    fs = slice(half * 256, (half + 1) * 256)
                    kb.dma("pool", wgs[s][:], wg[e_, :, fs].rearrange("(k p) n -> p k n", p=128), w=[u_wg[s]])
                    kb.dma("pool", wus[s][:], wu[e_, :, fs].rearrange("(k p) n -> p k n", p=128), w=[u_wu[s]])
                    kb.dma("pool", wds[s][:], wd[e_, fs, :].rearrange("(k p) n -> p k n", p=128), w=[u_wd[s]])
                    a = aT[eh % 2]; ua = u_aT[eh % 2]
                    for (t0, tn) in tbs:
                        for fc in range(2):
                            i = cnt % 2; cnt += 1
                            for k in range(16):
                                kb.op("pe", lambda e, k=k, i=i, fc=fc, t0=t0, tn=tn, s=s: e.matmul(
                                    pg[i][:, 0:tn], lhsT=wgs[s][:, k, fc * 128:(fc + 1) * 128], rhs=h2T[:, k, t0:t0 + tn],
                                    start=(k == 0), stop=(k == 15)), r=[u_wg[s]] + u_h2T, w=[u_pg[i]])
                            for k in range(16):
                                kb.op("pe", lambda e, k=k, i=i, fc=fc, t0=t0, tn=tn, s=s: e.matmul(
                                    pu[i][:, 0:tn], lhsT=wus[s][:, k, fc * 128:(fc + 1) * 128], rhs=h2T[:, k, t0:t0 + tn],
                                    start=(k == 0), stop=(k == 15)), r=[u_wu[s]] + u_h2T, w=[u_pu[i]])
                            kb.op("act", lambda e, i=i, tn=tn: e.activation(out=sg[i][:, 0:tn], in_=pg[i][:, 0:tn], func=AF.Silu),
                                  r=[u_pg[i]], w=[u_sg[i]])
                            kb.op("dve", lambda e, i=i, tn=tn, t0=t0, fc=fc, a=a: e.tensor_tensor(
                                out=a[:, fc, t0:t0 + tn], in0=sg[i][:, 0:tn], in1=pu[i][:, 0:tn], op=ALU.mult),
                                  r=[u_sg[i], u_pu[i]], w=[ua])
                    for qt in range(NQT):
                        for dc in range(4):
                            j = pyc % 4; pyc += 1
                            for fc in range(2):
                                kb.op("pe", lambda e, fc=fc, j=j, qt=qt, dc=dc, a=a, s=s: e.matmul(
                                    pyy[j][:], lhsT=a[:, fc, qt * 128:(qt + 1) * 128], rhs=wds[s][:, fc, dc * 512:(dc + 1) * 512],
                                    start=(fc == 0), stop=(fc == 1)), r=[ua, u_wd[s]], w=[u_pyy[j]])
                            dsl = slice(dc * 512, (dc + 1) * 512)
                            kb.op("dve", lambda e, j=j, qt=qt, dsl=dsl, e_=e_: e.scalar_tensor_tensor(
                                out=Y[:, qt, dsl], in0=pyy[j][:], scalar=gates[:, qt, e_:e_ + 1], in1=Y[:, qt, dsl],
                                op0=ALU.mult, op1=ALU.add), r=[u_pyy[j], u_gates[qt]], w=[u_Y[qt][dc]])
            kb.barrier()
            g2b = sb("g2b", [128, D], F32, p4); u_g2b = U()
            xr = [sb(f"xr4{i}", [128, D], F32, p4) for i in range(2)]; u_xr = [U(), U()]
            for qt in range(NQT):
                if qt == 0:
                    load_mod(g2b, 0, 5, u_g2b)
                if QT[qt] == 16:
                    load_mod(g2b, 1, 5, u_g2b)
                b = qt % 2
                kb.dma("sp", xr[b][:], x1d[qt * 128:(qt + 1) * 128, :], w=[u_xr[b]])
                kb.op("dve", lambda e, qt=qt: e.tensor_tensor(out=Y[:, qt, :], in0=Y[:, qt, :], in1=g2b[:], op=ALU.mult),
                      r=[u_g2b], w=u_Y[qt])
                kb.op("pool", lambda e, qt=qt, b=b: e.tensor_tensor(out=xr[b][:], in0=xr[b][:], in1=Y[:, qt, :], op=ALU.add),
                      r=u_Y[qt], w=[u_xr[b]])
                kb.dma("sp", xo[qt * 128:(qt + 1) * 128, :], xr[b][:], r=[u_xr[b]])
        kb.barrier()
    return nc


_PROG = {}


def _rope_tab(dim):
    t = np.arange(SEQ)
    r = (t // 64).astype(np.float32)
    col = (t % 64).astype(np.float32)
    nf = dim // 4
    inv = (np.float32(10000.0) ** (-np.arange(nf, dtype=np.float32) / nf)).astype(np.float32)
    ang = np.concatenate([r[:, None] * inv, col[:, None] * inv], axis=-1).astype(np.float32)
    return np.stack([np.cos(ang), np.sin(ang)]).astype(np.float32)


def _run_layer(l, last, x, xc, inp, debug=False):
    key = (bool(last), debug)
    if debug == "maps":
        nc = None
    else:
        if key not in _PROG:
            _PROG[key] = _layer_program(last, debug)
        nc = _PROG[key]
    f = np.float32
    ii = np.arange(128)
    maskP = (ii[None, :] <= ii[:, None]).astype(f)
    maskN = (ii[:, None] <= ii[None, :]).astype(f)
    lam_init = 0.8 - 0.6 * math.exp(-0.3 * l)
    ropes = {n: _rope_tab(d) for n, d in (("ropeA", 64), ("ropeB", 128), ("ropeC", 64))}
    in_maps = []
    for core in range(8):
        b, hq = core // 2, core % 2
        own = slice(hq * 1024, (hq + 1) * 1024)
        oth = slice((1 - hq) * 1024, (2 - hq) * 1024)
        perm = np.concatenate([np.arange(SEQ)[own], np.arange(SEQ)[oth]])
        xs = np.concatenate([x[b][perm], xc[b]], axis=0)
        cvec = np.stack([inp["c"][b], inp["c_ctx"]], axis=-1)
        cT = np.ascontiguousarray(cvec.reshape(16, 128, 2).transpose(1, 0, 2))
        zero = np.zeros((128, 128), f)
        masks = np.stack([maskP, maskN, maskP if hq == 1 else zero, maskN if hq == 0 else zero])
        m = {
            "xs": np.ascontiguousarray(xs, dtype=f), "cT": cT.astype(f),
            "ada_w": inp["ada_w"][l], "ada_b": inp["ada_b"][l][None], "norm1_g": inp["norm1_g"][l][None],
            "norm2_g": inp["norm2_g"][l][None], "w_in": inp["w_in"][l], "w_out": inp["w_out"][l],
            "a_qn": inp["a_qn"][l][None], "a_kn": inp["a_kn"][l][None], "a_lambda": inp["a_lambda"][l].reshape(1, 256),
            "a_subln": inp["a_subln"][l][None], "b_qn": inp["b_qn"][l][None], "b_kn": inp["b_kn"][l][None],
            "b_sink": inp["b_sink"][l][None], "c_qa_norm": inp["c_qa_norm"][l][None],
            "c_kva_norm": inp["c_kva_norm"][l][None], "c_wuq": inp["c_wuq"][l], "c_wukv": inp["c_wukv"][l],
            "c_qn": inp["c_qn"][l][None], "c_kn": inp["c_kn"][l][None], "router_w": inp["router_w"],
            "router_bias": inp["router_bias"][None], "moe_w_gate": inp["moe_w_gate"][l], "moe_w_up": inp["moe_w_up"][l],
            "moe_w_down": inp["moe_w_down"][l], "ident": np.eye(128, dtype=f), "masks": masks,
            "laminit": np.array([[lam_init, 1.0 - lam_init]], f),
        }
        for n, tab in ropes.items():
            m[n] = np.ascontiguousarray(tab[:, perm, :])
        in_maps.append({k: np.ascontiguousarray(v, dtype=f) for k, v in m.items()})
    if debug == "maps":
        return in_maps
    res = run_bass_kernel_spmd(nc, in_maps, core_ids=list(range(8)))
    xn = np.empty_like(x)
    xcn = np.empty_like(xc)
    for core in range(8):
        b, hq = core // 2, core % 2
        o = res.results[core]["xo"]
        xn[b, hq * 1024:(hq + 1) * 1024] = o[0:1024]
        if not last and hq == 0:
            xcn[b] = o[1024:1280]
    if debug:
        return xn, xcn, res
    return xn, xcn


def kernel(**inputs):
    inp = {k: np.asarray(v) for k, v in inputs.items()}
    x = np.ascontiguousarray(inp["x"], dtype=np.float32)
    xc = np.ascontiguousarray(inp["ctx"], dtype=np.float32)
    for l in range(DEPTH):
        x, xc = _run_layer(l, l == DEPTH - 1, x, xc, inp)
    return x
```
